# Optimizing a Trainium2 kernel written in Bass

```python
import math
import jax, jax.numpy as jnp
from jax import lax
import numpy as np

D_MODEL = 4096
BATCH = 2
SEQ = 8192
DEPTH = 1

CONV_WIDTH = 2048
CONV_K = 3
N_HEADS = 16
N_KV_HEADS = 4
HEAD_DIM = 128
ATTN_WIDTH = N_HEADS * HEAD_DIM
KV_WIDTH = N_KV_HEADS * HEAD_DIM
WINDOW = 128
BLOCK = 128
MEM_LEN = 256
N_CROSS_HEADS = 4
CROSS_HEAD_DIM = 128
CROSS_WIDTH = N_CROSS_HEADS * CROSS_HEAD_DIM
N_EXPERTS = 16
EXPERT_FF = 2048
CAPACITY_FACTOR = 2
RMS_EPS = 1e-6
NEG_INF = -1e30

OFF_B = 0
OFF_C = OFF_B + CONV_WIDTH
OFF_U = OFF_C + CONV_WIDTH
OFF_Q = OFF_U + CONV_WIDTH
OFF_K = OFF_Q + ATTN_WIDTH
OFF_V = OFF_K + KV_WIDTH
OFF_GA = OFF_V + KV_WIDTH
OFF_GB = OFF_GA + D_MODEL
IN_COLS = OFF_GB + D_MODEL

kernel_name = "hybrid_gated_conv_swa_ec_moe_encoder"


def rmsnorm(x, g):
    xf = x.astype(jnp.float32)
    y = xf * lax.rsqrt(jnp.mean(xf * xf, axis=-1, keepdims=True) + RMS_EPS)
    return (y * g.astype(jnp.float32)).astype(x.dtype)


def alibi_slopes(n_heads):
    return jnp.power(2.0, -8.0 * (jnp.arange(n_heads, dtype=jnp.float32) + 1.0) / n_heads)


def short_conv_mixer(bg, cg, u, conv_w):
    z = cg * u
    zp = jnp.pad(z, ((0, 0), (1, 1), (0, 0)))
    w = conv_w.astype(z.dtype)
    conv = w[0] * zp[:, :-2] + w[1] * zp[:, 1:-1] + w[2] * zp[:, 2:]
    return bg * conv


def windowed_gqa(q, k, v, sinks):
    bsz, s_len, _, dh = q.shape
    nb = s_len // BLOCK
    g = N_HEADS // N_KV_HEADS
    qb = q.reshape(bsz, nb, BLOCK, N_KV_HEADS, g, dh)

    def band(t):
        tp = jnp.pad(t, ((0, 0), (BLOCK, BLOCK), (0, 0), (0, 0)))
        tp = tp.reshape(bsz, nb + 2, BLOCK, N_KV_HEADS, dh)
        return jnp.concatenate([tp[:, :-2], tp[:, 1:-1], tp[:, 2:]], axis=2)

    kb, vb = band(k), band(v)
    scale = 1.0 / math.sqrt(dh)
    s = jnp.einsum('bnqkgd,bnskd->bnkgqs', qb, kb, preferred_element_type=jnp.float32) * scale

    qpos = jnp.arange(s_len, dtype=jnp.int32).reshape(nb, BLOCK)
    kpos = (jnp.arange(nb, dtype=jnp.int32)[:, None] * BLOCK - BLOCK
            + jnp.arange(3 * BLOCK, dtype=jnp.int32)[None, :])
    dist = jnp.abs(qpos[:, :, None] - kpos[:, None, :])
    valid = (dist <= WINDOW) & (kpos >= 0)[:, None, :] & (kpos < s_len)[:, None, :]
    slopes = alibi_slopes(N_HEADS).reshape(N_KV_HEADS, g)
    bias = -slopes[None, :, :, None, None] * dist.astype(jnp.float32)[:, None, None, :, :]
    s = jnp.where(valid[:, None, None], s + bias[None], NEG_INF)

    sink = jnp.broadcast_to(sinks.astype(jnp.float32).reshape(1, 1, N_KV_HEADS, g, 1, 1),
                            s.shape[:-1] + (1,))
    p = jax.nn.softmax(jnp.concatenate([s, sink], axis=-1), axis=-1)[..., :-1]
    o = jnp.einsum('bnkgqs,bnskd->bnqkgd', p.astype(v.dtype), vb)
    return o.reshape(bsz, s_len, N_HEADS * dh)


def memory_cross_attention(h, mem_n, w_q, w_kv, w_o):
    bsz, s_len, _ = h.shape
    q = (h @ w_q).reshape(bsz, s_len, N_CROSS_HEADS, CROSS_HEAD_DIM)
    kv = mem_n @ w_kv
    k = kv[..., :CROSS_WIDTH].reshape(bsz, -1, N_CROSS_HEADS, CROSS_HEAD_DIM)
    v = kv[..., CROSS_WIDTH:].reshape(bsz, -1, N_CROSS_HEADS, CROSS_HEAD_DIM)
    s = jnp.einsum('bshd,bmhd->bhsm', q, k, preferred_element_type=jnp.float32) / math.sqrt(CROSS_HEAD_DIM)
    p = jax.nn.softmax(s, axis=-1)
    o = jnp.einsum('bhsm,bmhd->bshd', p.astype(v.dtype), v).reshape(bsz, s_len, CROSS_WIDTH)
    return o @ w_o


def expert_choice_moe(h, w_router, w_gate_e, w_up_e, w_down_e):
    bsz, t_len, d = h.shape
    cap = CAPACITY_FACTOR * t_len // N_EXPERTS
    logits = jnp.einsum('btd,de->bte', h, w_router, preferred_element_type=jnp.float32)
    aff = jax.nn.softmax(logits, axis=-1)
    gate, idx = lax.top_k(jnp.swapaxes(aff, 1, 2), cap)
    xg = jax.vmap(lambda hb, ib: hb[ib])(h, idx)
    a = jnp.einsum('becd,edf->becf', xg, w_gate_e)
    u = jnp.einsum('becd,edf->becf', xg, w_up_e)
    y = jnp.einsum('becf,efd->becd', jax.nn.silu(a) * u, w_down_e)
    y = y * gate[..., None].astype(y.dtype)
    return jax.vmap(lambda yb, ib: jax.ops.segment_sum(
        yb.reshape(-1, d), ib.reshape(-1), num_segments=t_len))(y, idx)


def setup_inputs(seed: int = 0) -> dict:
    key = jax.random.key(seed)
    ks = jax.random.split(key, 24)
    f32 = jnp.float32

    def w(k, shape, fan_in):
        return jax.random.normal(k, shape, f32) * (fan_in ** -0.5)

    def gain(k, n):
        return jnp.ones((n,), f32) + 0.02 * jax.random.normal(k, (n,), f32)

    return {
        "x": jax.random.normal(ks[0], (BATCH, SEQ, D_MODEL), f32),
        "mem": jax.random.normal(ks[1], (BATCH, MEM_LEN, D_MODEL), f32),
        "g_mix": gain(ks[2], D_MODEL),
        "w_in": w(ks[3], (D_MODEL, IN_COLS), D_MODEL),
        "conv_w": w(ks[4], (CONV_K, CONV_WIDTH), CONV_K),
        "attn_sinks": 0.5 * jax.random.normal(ks[5], (N_HEADS,), f32),
        "w_conv_out": w(ks[6], (CONV_WIDTH, D_MODEL), CONV_WIDTH),
        "w_attn_out": w(ks[7], (ATTN_WIDTH, D_MODEL), ATTN_WIDTH),
        "w_out": w(ks[8], (D_MODEL, D_MODEL), D_MODEL),
        "g_cross": gain(ks[9], D_MODEL),
        "g_mem": gain(ks[10], D_MODEL),
        "w_q_cross": w(ks[11], (D_MODEL, CROSS_WIDTH), D_MODEL),
        "w_kv_cross": w(ks[12], (D_MODEL, 2 * CROSS_WIDTH), D_MODEL),
        "w_o_cross": w(ks[13], (CROSS_WIDTH, D_MODEL), CROSS_WIDTH),
        "g_moe": gain(ks[14], D_MODEL),
        "w_router": w(ks[15], (D_MODEL, N_EXPERTS), D_MODEL),
        "w_gate_e": w(ks[16], (N_EXPERTS, D_MODEL, EXPERT_FF), D_MODEL),
        "w_up_e": w(ks[17], (N_EXPERTS, D_MODEL, EXPERT_FF), D_MODEL),
        "w_down_e": w(ks[18], (N_EXPERTS, EXPERT_FF, D_MODEL), EXPERT_FF),
        "g_final": gain(ks[19], D_MODEL),
    }


def reference(x, mem, g_mix, w_in, conv_w, attn_sinks, w_conv_out, w_attn_out, w_out,
              g_cross, g_mem, w_q_cross, w_kv_cross, w_o_cross,
              g_moe, w_router, w_gate_e, w_up_e, w_down_e, g_final):
    bsz, s_len, _ = x.shape
    mem_n = rmsnorm(mem, g_mem)
    for _layer in range(DEPTH):
        h = rmsnorm(x, g_mix)
        p = h @ w_in
        bg = p[..., OFF_B:OFF_C]
        cg = p[..., OFF_C:OFF_U]
        u = p[..., OFF_U:OFF_Q]
        q = p[..., OFF_Q:OFF_K].reshape(bsz, s_len, N_HEADS, HEAD_DIM)
        k = p[..., OFF_K:OFF_V].reshape(bsz, s_len, N_KV_HEADS, HEAD_DIM)
        v = p[..., OFF_V:OFF_GA].reshape(bsz, s_len, N_KV_HEADS, HEAD_DIM)
        gate_a = jax.nn.sigmoid(p[..., OFF_GA:OFF_GB])
        gate_b = jax.nn.sigmoid(p[..., OFF_GB:IN_COLS])

        y_a = short_conv_mixer(bg, cg, u, conv_w) @ w_conv_out
        y_b = windowed_gqa(q, k, v, attn_sinks) @ w_attn_out
        x = x + (gate_a * y_a + gate_b * y_b) @ w_out

        x = x + memory_cross_attention(rmsnorm(x, g_cross), mem_n, w_q_cross, w_kv_cross, w_o_cross)

        x = x + expert_choice_moe(rmsnorm(x, g_moe), w_router, w_gate_e, w_up_e, w_down_e)
    return rmsnorm(x, g_final)
```

```python
import numpy as np
import concourse.bass as bass
import concourse.mybir as mybir
from concourse.bass_utils import run_bass_kernel_spmd

F32 = mybir.dt.float32
BF16 = mybir.dt.bfloat16
AF = mybir.ActivationFunctionType
ALU = mybir.AluOpType

NCORES = 8
D = 4096
KC = 32
TOK = 2048
T = 512
NT = TOK // T
W = T + 256
SEQ = 8192
NE = 16
FF = 2048
CAP = 1024
EPS = 1e-6
SCALE = 1.0 / float(np.sqrt(128.0))
NBIS = 34


class Lane:
    def __init__(self, sem, step):
        self.sem = sem
        self.n = 0
        self.step = step


class Sched:
    def __init__(self, nc):
        self.nc = nc
        self.eng = {"pe": nc.tensor, "act": nc.scalar, "dve": nc.vector, "pool": nc.gpsimd, "sp": nc.sync}
        self.lanes = {}
        self.seen = {e: {} for e in self.eng}
        self.st = {}
        self.prog = {e: [] for e in self.eng}

    def flush(self, blk):
        for e, nm in (("sp", "sync"), ("pool", "gpsimd"), ("pe", "tensor"), ("act", "scalar"), ("dve", "vector")):
            lst = self.prog[e]
            if not lst:
                continue

            def body(eng, lst=lst):
                for th in lst:
                    th(eng)

            getattr(blk, nm)(body)
            self.prog[e] = []

    def add_lane(self, name, sem, step):
        self.lanes[name] = Lane(sem, step)

    def _deps(self, reads, writes):
        d = {}

        def add(x):
            if x is not None:
                d[x[0]] = max(d.get(x[0], 0), x[1])

        for k in reads:
            st = self.st.get(k)
            if st:
                add(st[0])
        for k in writes:
            st = self.st.get(k)
            if st:
                add(st[0])
                for l, q in st[1].items():
                    add((l, q))
        return d

    def _wait(self, e, d):
        for l, q in d.items():
            if l == "pe" and e == "pe":
                continue
            if self.seen[e].get(l, 0) >= q:
                continue
            if q > self.lanes[l].n:
                import traceback
                raise RuntimeError(f"future-signal wait: engine {e} on lane {l} value {q} > {self.lanes[l].n}")
            self.prog[e].append(lambda eng, sm=self.lanes[l].sem, q=q: eng.wait_ge(sm, q))
            self.seen[e][l] = q

    def _mark(self, lane, seq, reads, writes):
        for k in reads:
            st = self.st.setdefault(k, [None, {}])
            st[1][lane] = max(st[1].get(lane, 0), seq)
        for k in writes:
            self.st[k] = [(lane, seq), {}]

    def op(self, e, fn, reads=(), writes=(), sig=True):
        self._wait(e, self._deps(reads, writes))
        L = self.lanes[e]
        if sig:
            L.n += 1
            self.prog[e].append(lambda eng, fn=fn, sm=L.sem: fn(eng).then_inc(sm, 1))
            seq = L.n
        else:
            self.prog[e].append(lambda eng, fn=fn: fn(eng))
            seq = L.n + 1
        self._mark(e, seq, reads, writes)

    def dma(self, q, lane, out, in_, reads=(), writes=(), **kw):
        self._wait(q, self._deps(reads, writes))
        L = self.lanes[lane]
        L.n += 16
        self.prog[q].append(lambda eng, out=out, in_=in_, kw=kw, sm=L.sem: eng.dma_start(out=out, in_=in_, **kw).then_inc(sm, 16))
        self._mark(lane, L.n, reads, writes)

    def seal(self, lane, keys):
        n = self.lanes[lane].n
        for k in keys:
            st = self.st.get(k)
            if st and st[0] and st[0][0] == lane:
                st[0] = (lane, n)

    def cc(self, fn, writes):
        L = self.lanes["cc"]
        L.n += 1
        self.prog["pool"].append(lambda eng, fn=fn, sm=L.sem: fn(eng).then_inc(sm, 1))
        self._mark("cc", L.n, [], writes)

    def barrier(self, engines=("pe", "act", "dve", "pool", "sp")):
        for e in engines:
            d = {l: L.n for l, L in self.lanes.items() if L.n > 0}
            self._wait(e, d)


def build_nc():
    from contextlib import ExitStack
    nc = bass.Bass("TRN2", target_bir_lowering=False)

    def din(name, shape, dt=F32):
        return nc.dram_tensor(name, list(shape), dt, kind="ExternalInput").ap()

    def dint(name, shape, dt=F32):
        return nc.dram_tensor(name, list(shape), dt, kind="Internal").ap()

    xT = din("xT", [D, TOK + 256])
    memT = din("memT", [D, 256])
    gains = din("gains", [128, 4 * KC])
    gmem = din("gmem", [128, KC])
    convw = din("convw", [128, 48])
    sinks = din("sinks", [128, 16])
    etab = din("etab", [128, 3 * 16 * 128], BF16)
    eedge = din("eedge", [128, 2 * 16 * 128], BF16)
    wrt = din("wrt", [128, KC * 16])
    selm = din("selm", [16, 16 * 128])
    gmat = din("gmat", [128, 128])
    bsel_d = din("bsel", [16, 1])
    sh = {
        "win": din("win_sh", [17 * 128, 4096]),
        "wco": din("wco_sh", [4 * 128, 2048]),
        "wao": din("wao_sh", [4 * 128, 2048]),
        "wout": din("wout_sh", [4 * 128, 4096]),
        "wq": din("wq_sh", [128, 2048]),
        "wkv": din("wkv_sh", [128, 4096]),
        "wo": din("wo_sh", [4 * 128, 512]),
        "wg": din("wg_sh", [16 * 256, 4096]),
        "wu": din("wu_sh", [16 * 256, 4096]),
        "wd": din("wd_sh", [16 * 512, 2048]),
    }
    outT = nc.dram_tensor("outT", [D, TOK], F32, kind="ExternalOutput").ap()
    it = {k: dint(k + "_i", v.shape, BF16) for k, v in sh.items()}
    UPP = {"win": 8, "wco": 8, "wao": 8, "wout": 8, "wo": 8, "wq": 4, "wkv": 8, "wg": 16, "wu": 16, "wd": 32}
    NPC = {"win": 17, "wco": 4, "wao": 4, "wout": 4, "wo": 4, "wq": 1, "wkv": 1, "wg": 16, "wu": 16, "wd": 16}
    XW = {"win": 4096, "wco": 2048, "wao": 2048, "wout": 4096, "wo": 512, "wq": 2048, "wkv": 4096, "wg": 4096, "wu": 4096, "wd": 2048}
    RPP = {k: (UPP[k] * 128 if k != "wq" else 8 * 128) for k in UPP}
    full = {k: [dint(f"{k}_f{i}", [RPP[k], XW[k]], BF16) for i in range(NPC[k])] for k in UPP}
    x1s = dint("x1s", [D, TOK])
    x2s = dint("x2s", [D, TOK])
    h3s = dint("h3s", [D, TOK], BF16)
    aff_loc = dint("aff_loc", [16, TOK])
    aff_all = dint("aff_all", [NCORES * 16, TOK])

    es = ExitStack()
    with es:
        def sb(name, shape, dt=F32):
            return es.enter_context(nc.sbuf_tensor(name, list(shape), dt))

        def sem(name):
            return es.enter_context(nc.semaphore(name))

        S = Sched(nc)
        for e in ("pe", "act", "dve"):
            S.add_lane(e, sem("s_" + e), 1)
        S.add_lane("cc", sem("s_cc"), 1)

        def dlane(name):
            S.add_lane(name, sem("l_" + name), 16)
            return name

        NW = 3
        wsl = [sb(f"wsl{i}", [128, 4096], BF16) for i in range(NW)]
        for i in range(NW):
            dlane(f"w{i}")
        ps = [es.enter_context(nc.psum_tensor(f"ps{i}", [128, 512], F32)) for i in range(8)]
        g_sb = sb("g_sb", [128, 4 * KC])
        gm_sb = sb("gm_sb", [128, KC])
        cw_sb = sb("cw_sb", [128, 48])
        snk = sb("snk", [128, 16])
        ones_f = sb("ones_f", [128, 128])
        ones_b = sb("ones_b", [128, 128], BF16)
        wr_sb = sb("wr_sb", [128, KC * 16])
        gmat_sb = sb("gmat_sb", [128, 128])
        for nm in ("cst", "cp", "misc"):
            dlane(nm)
        blk = es.enter_context(nc.Block())
        state = {"wi": 0}

        def PSK(i):
            return f"ps{i}"

        for (dst, src, key) in ((g_sb, gains, "g"), (gm_sb, gmem, "gm"), (cw_sb, convw, "cw"), (snk, sinks, "snk"),
                                (wr_sb, wrt, "wr"), (gmat_sb, gmat, "gmat")):
            S.dma("sp", "cst", dst[:], src, writes=[key])
        S.seal("cst", ["g", "gm", "cw", "snk", "wr", "gmat"])
        S.op("dve", lambda e: e.memset(ones_f[:], 1.0), writes=["ones_f"])
        S.op("dve", lambda e: e.memset(ones_b[:], 1.0), writes=["ones_b"])

        for k in sh:
            rows = sh[k].shape[0]
            step = 1024
            for r0 in range(0, rows, step):
                r1 = min(rows, r0 + step)
                S.dma("pool", "cp", it[k][r0:r1, :], sh[k][r0:r1, :], writes=[f"it_{k}_{r0}"], max_dma_last_dim=8192)
        S._wait("pool", {"cp": S.lanes["cp"].n})

        def cc_op(k, i):
            nr = RPP[k] // NCORES
            S.cc(lambda e: e.collective_compute("AllGather", ALU.bypass, replica_groups=[list(range(NCORES))],
                                                ins=[it[k][i * nr:(i + 1) * nr, :]], outs=[full[k][i][:, :]]), [f"{k}.{i}"])

        cc_op("wkv", 0)
        for i in (8, 2, 3, 4, 5, 0, 1, 6, 7):
            cc_op("win", i)
        for i in range(4):
            cc_op("win", 9 + i)
            cc_op("win", 13 + i)
            cc_op("wco", i)
            cc_op("wao", i)
        for i in range(4):
            cc_op("wout", i)
        cc_op("wq", 0)
        for i in range(4):
            cc_op("wo", i)
        NEARLY = 4
        for e_ in range(NEARLY):
            cc_op("wg", e_)
            cc_op("wu", e_)
            cc_op("wd", e_)

        def load_unit(k, u):
            i = state["wi"] % NW
            state["wi"] += 1
            key = f"wsl{i}"
            pc, ul = u // UPP[k], u % UPP[k]
            if k == "wq":
                src = full[k][pc][ul * 256:(ul + 1) * 256, :].rearrange("(h p) x -> p h x", p=128)
                dst = wsl[i][:, 0:4096].rearrange("p (h x) -> p h x", h=2)
            else:
                src = full[k][pc][ul * 128:(ul + 1) * 128, :]
                dst = wsl[i][:, 0:XW[k]]
            S.dma("pool", f"w{i}", dst, src, reads=[f"{k}.{pc}"], writes=[key])
            return wsl[i], key

        def rms_finish(pa, oa, pk, rkey):
            S.op("dve", lambda e: e.tensor_scalar(out=oa, in0=pa, scalar1=1.0 / D, scalar2=EPS, op0=ALU.mult, op1=ALU.add),
                 reads=[pk], writes=[rkey])
            S.op("act", lambda e: e.activation(out=oa, in_=oa, func=AF.Sqrt), reads=[rkey], writes=[rkey])
            S.op("dve", lambda e: e.reciprocal(out=oa, in_=oa), reads=[rkey], writes=[rkey])

        def mm_unit(wt, wkey, nk, rhs_fn, rkeys_fn, bank, ncols):
            for k in range(nk):
                S.op("pe", lambda e, k=k: e.matmul(ps[bank][:, 0:ncols], lhsT=wt[:, k * 128:(k + 1) * 128],
                                                    rhs=rhs_fn(k), start=(k == 0), stop=(k == nk - 1)),
                     reads=[wkey] + rkeys_fn(k), writes=[PSK(bank)], sig=(k == nk - 1))

        es2 = ExitStack()
        with es2:
            def sb2(name, shape, dt=F32):
                return es2.enter_context(nc.sbuf_tensor(name, list(shape), dt))

            hTc = sb2("hTc", [128, KC, T], BF16)
            yAT = sb2("yAT", [128, 16, T], BF16)
            oT = sb2("oT", [128, 16, T], BF16)
            R = sb2("R", [128, 18432], BF16)
            qTg = [R[:, 0:2048].rearrange("p (a b c) -> p a b c", a=4, b=4), R[:, 2048:4096].rearrange("p (a b c) -> p a b c", a=4, b=4)]
            kT = R[:, 4096:7168].rearrange("p (a b c) -> p a b c", a=4, b=6)
            vtok = R[:, 7168:10240].rearrange("p (a b) -> p a b", a=6)
            hTh = R[:, 10240:18432].rearrange("p (a b) -> p a b", a=KC)
            mT = R[:, 0:16384].rearrange("p (a b) -> p a b", a=KC)
            RK = ["qTg0", "qTg1", "kT", "vtok"] + [f"hTh.{c}" for c in range(KC)]
            etb = sb2("etb", [128, 3, 16, 128], BF16)
            eeb = sb2("eeb", [128, 16, 128], BF16)
            esn = sb2("esn", [128, 16])
            kcT = sb2("kcT", [128, 4, 256], BF16)
            vc = sb2("vc", [128, 2, 512], BF16)
            qcT = sb2("qcT", [128, 4, T], BF16)
            ocT = sb2("ocT", [128, 4, T], BF16)
            rstd = sb2("rstd", [128, W])
            NX = 2
            xs = [sb2(f"xs{i}", [128, W]) for i in range(NX)]
            sq = [sb2(f"sq{i}", [128, W]) for i in range(NX)]
            for i in range(NX):
                dlane(f"xs{i}")
            tA = [sb2(f"tA{i}", [128, 514]) for i in range(2)]
            tB = [sb2(f"tB{i}", [128, 514]) for i in range(2)]
            tC = [sb2(f"tC{i}", [128, 512]) for i in range(2)]
            tD = [sb2(f"tD{i}", [128, 512]) for i in range(2)]
            chb = [sb2(f"chb{i}", [128, 2]) for i in range(2)]
            pT = [sb2(f"pT{i}", [128, 512], BF16) for i in range(2)]
            NO = 2
            xo = [sb2(f"xo{i}", [128, T]) for i in range(NO)]
            hb = [sb2(f"hb{i}", [128, T], BF16) for i in range(NO)]
            for i in range(NO):
                dlane(f"xo{i}")
                dlane(f"hb{i}")
            lgs = sb2("lgs", [16, T])
            afft = sb2("afft", [16, T])
            dlane("afft")
            dlane("eeb")
            cnt = {"x": 0, "t": 0, "o": 0}

            S.dma("sp", "cst", etb[:].rearrange("p a b c -> p (a b c)"), etab, writes=["etb"])
            S.seal("cst", ["g", "gm", "cw", "snk", "wr", "gmat", "etb"])
            S.op("act", lambda e: e.activation(out=esn[:], in_=snk[:], func=AF.Exp), reads=["snk"], writes=["esn"])

            def norm_stream(src_fn, width, out_fn, ssbanks):
                for c in range(KC):
                    i = cnt["x"] % NX
                    cnt["x"] += 1
                    S.dma("sp", f"xs{i}", xs[i][:, 0:width], src_fn(c), writes=[f"xs{i}"])
                    S.op("act", lambda e, i=i: e.activation(out=sq[i][:, 0:width], in_=xs[i][:, 0:width], func=AF.Square),
                         reads=[f"xs{i}"], writes=[f"sq{i}"])
                    off = 0
                    for (bk, n) in ssbanks:
                        S.op("pe", lambda e, i=i, bk=bk, n=n, off=off, c=c: e.matmul(
                            ps[bk][:, 0:n], lhsT=ones_f[:], rhs=sq[i][:, off:off + n], start=(c == 0), stop=(c == KC - 1)),
                            reads=[f"sq{i}", "ones_f"], writes=[PSK(bk)])
                        off += n
                off = 0
                for (bk, n) in ssbanks:
                    rms_finish(ps[bk][:, 0:n], rstd[:, off:off + n], PSK(bk), f"rstd{off}")
                    off += n
                for c in range(KC):
                    i = cnt["x"] % NX
                    cnt["x"] += 1
                    S.dma("sp", f"xs{i}", xs[i][:, 0:width], src_fn(c), writes=[f"xs{i}"])
                    out_fn(c, i)

            memn = mT

            def mem_out(c, i):
                S.op("dve", lambda e: e.scalar_tensor_tensor(out=memn[:, c, 0:256], in0=xs[i][:, 0:256], scalar=gm_sb[:, c:c + 1],
                                                            in1=rstd[:, 0:256], op0=ALU.mult, op1=ALU.mult),
                     reads=[f"xs{i}", "gm", "rstd0"], writes=[f"mT.{c}"])

            norm_stream(lambda c: memT[c * 128:(c + 1) * 128, :], 256, mem_out, [(6, 256)])
            for h in range(4):
                wt, wk = load_unit("wkv", h)
                mm_unit(wt, wk, KC, lambda k: memn[:, k, 0:256], lambda k: [f"mT.{k}", "mTall"], h % 2, 256)
                S.op("act", lambda e, h=h: e.activation(out=kcT[:, h, :], in_=ps[h % 2][:, 0:256], func=AF.Copy),
                     reads=[PSK(h % 2)], writes=["kcT"])
            for h in range(4):
                wt, wk = load_unit("wkv", 4 + h)
                for mb in range(2):
                    bank = 2 + mb
                    for k in range(KC):
                        S.op("pe", lambda e, k=k, mb=mb, bank=bank, wt=wt: e.matmul(
                            ps[bank][:, 0:128], lhsT=memn[:, k, mb * 128:(mb + 1) * 128], rhs=wt[:, k * 128:(k + 1) * 128],
                            start=(k == 0), stop=(k == KC - 1)), reads=[wk, f"mT.{k}", "mTall"], writes=[PSK(bank)], sig=(k == KC - 1))
                    S.op("dve", lambda e, mb=mb, h=h, bank=bank: e.tensor_copy(out=vc[:, mb, h * 128:(h + 1) * 128], in_=ps[bank][:, 0:128]),
                         reads=[PSK(bank)], writes=["vc"])

            for j in range(NT):
                c0 = j * T
                tcol = slice(j * T, (j + 1) * T)
                if j == 0:
                    S.dma("sp", "eeb", eeb[:].rearrange("p b c -> p (b c)"), eedge[:, 0:2048], writes=["eeb"])
                if j == NT - 1:
                    S.dma("sp", "eeb", eeb[:].rearrange("p b c -> p (b c)"), eedge[:, 2048:4096], writes=["eeb"])

                def h_out(c, i):
                    S.op("dve", lambda e: e.scalar_tensor_tensor(out=hTc[:, c, :], in0=xs[i][:, 128:128 + T], scalar=g_sb[:, c:c + 1],
                                                                in1=rstd[:, 128:128 + T], op0=ALU.mult, op1=ALU.mult),
                         reads=[f"xs{i}", "g", "rstd0", "rstd512"], writes=[f"hTc.{c}"])
                    S.op("dve", lambda e: e.scalar_tensor_tensor(out=hTh[:, c, 0:128], in0=xs[i][:, 0:128], scalar=g_sb[:, c:c + 1],
                                                                in1=rstd[:, 0:128], op0=ALU.mult, op1=ALU.mult),
                         reads=[f"xs{i}", "g", "rstd0"], writes=[f"hTh.{c}", "mTall"])
                    S.op("dve", lambda e: e.scalar_tensor_tensor(out=hTh[:, c, 128:256], in0=xs[i][:, 128 + T:W], scalar=g_sb[:, c:c + 1],
                                                                in1=rstd[:, 128 + T:W], op0=ALU.mult, op1=ALU.mult),
                         reads=[f"xs{i}", "g", "rstd512"], writes=[f"hTh.{c}", "mTall"])

                norm_stream(lambda c: xT[c * 128:(c + 1) * 128, c0:c0 + W], W, h_out, [(6, 512), (7, 256)])

                hc = lambda k: hTc[:, k, :]
                hck = lambda k: [f"hTc.{k}"]
                hhk = lambda k: [f"hTh.{k}", "Rall"]
                pair = [0]

                def next_pair():
                    p = pair[0] % 3
                    pair[0] += 1
                    return 2 * p, 2 * p + 1

                for kv in range(4):
                    wt, wk = load_unit("win", 64 + kv)
                    bc, bh = next_pair()
                    mm_unit(wt, wk, KC, hc, hck, bc, T)
                    mm_unit(wt, wk, KC, lambda k: hTh[:, k, :], hhk, bh, 256)
                    S.op("act", lambda e, kv=kv, bc=bc: e.activation(out=kT[:, kv, 1:5, :], in_=ps[bc][:, :].rearrange("p (a b) -> p a b", a=4), func=AF.Copy),
                         reads=[PSK(bc)], writes=["kT", "mTall"])
                    S.op("act", lambda e, kv=kv, bh=bh: e.activation(out=kT[:, kv, 0, :], in_=ps[bh][:, 0:128], func=AF.Copy),
                         reads=[PSK(bh)], writes=["kT"])
                    S.op("act", lambda e, kv=kv, bh=bh: e.activation(out=kT[:, kv, 5, :], in_=ps[bh][:, 128:256], func=AF.Copy),
                         reads=[PSK(bh)], writes=["kT"])
                for kv in range(4):
                    wt, wk = load_unit("win", 68 + kv)
                    bc, bh = next_pair()
                    for wb in range(6):
                        if wb == 0:
                            lf = lambda k: hTh[:, k, 0:128]
                            lk = hhk
                        elif wb == 5:
                            lf = lambda k: hTh[:, k, 128:256]
                            lk = hhk
                        else:
                            lf = lambda k, wb=wb: hTc[:, k, (wb - 1) * 128:wb * 128]
                            lk = hck
                        bank, col = (bc, wb * 128) if wb < 4 else (bh, (wb - 4) * 128)
                        for k in range(KC):
                            S.op("pe", lambda e, k=k, lf=lf, bank=bank, col=col, wt=wt: e.matmul(
                                ps[bank][:, col:col + 128], lhsT=lf(k), rhs=wt[:, k * 128:(k + 1) * 128],
                                start=(k == 0), stop=(k == KC - 1)), reads=[wk] + lk(k), writes=[PSK(bank)], sig=(k == KC - 1))
                    S.op("dve", lambda e, kv=kv, bc=bc: e.tensor_copy(out=vtok[:, 0:4, kv * 128:(kv + 1) * 128],
                                                                      in_=ps[bc][:, :].rearrange("p (a b) -> p a b", a=4)),
                         reads=[PSK(bc)], writes=["vtok", "mTall"])
                    S.op("dve", lambda e, kv=kv, bh=bh: e.tensor_copy(out=vtok[:, 4:6, kv * 128:(kv + 1) * 128],
                                                                      in_=ps[bh][:, 0:256].rearrange("p (a b) -> p a b", a=2)),
                         reads=[PSK(bh)], writes=["vtok"])
                for ch in range(16):
                    a = cnt["t"] % 2
                    cnt["t"] += 1
                    wt, wk = load_unit("win", 16 + ch)
                    bc, bh = next_pair()
                    mm_unit(wt, wk, KC, hc, hck, bc, T)
                    mm_unit(wt, wk, KC, lambda k: hTh[:, k, 127:129], hhk, bh, 2)
                    S.op("act", lambda e, a=a, bc=bc: e.activation(out=tC[a][:], in_=ps[bc][:, :], func=AF.Copy),
                         reads=[PSK(bc)], writes=[f"tC{a}"])
                    S.op("act", lambda e, a=a, bh=bh: e.activation(out=chb[a][:], in_=ps[bh][:, 0:2], func=AF.Copy),
                         reads=[PSK(bh)], writes=[f"chb{a}"])
                    wt, wk = load_unit("win", 32 + ch)
                    bc, bh = next_pair()
                    mm_unit(wt, wk, KC, hc, hck, bc, T)
                    mm_unit(wt, wk, KC, lambda k: hTh[:, k, 127:129], hhk, bh, 2)
                    S.op("dve", lambda e, a=a, bc=bc: e.tensor_tensor(out=tA[a][:, 1:513], in0=ps[bc][:, :], in1=tC[a][:], op=ALU.mult),
                         reads=[PSK(bc), f"tC{a}"], writes=[f"tA{a}"])
                    S.op("dve", lambda e, a=a, bh=bh: e.tensor_tensor(out=tA[a][:, 0:1], in0=ps[bh][:, 0:1], in1=chb[a][:, 0:1], op=ALU.mult),
                         reads=[PSK(bh), f"chb{a}", f"tA{a}"], writes=[f"tA{a}"])
                    S.op("dve", lambda e, a=a, bh=bh: e.tensor_tensor(out=tA[a][:, 513:514], in0=ps[bh][:, 1:2], in1=chb[a][:, 1:2], op=ALU.mult),
                         reads=[PSK(bh), f"chb{a}", f"tA{a}"], writes=[f"tA{a}"])
                    S.op("dve", lambda e, a=a, ch=ch: e.tensor_scalar(out=tB[a][:, 0:512], in0=tA[a][:, 0:512], scalar1=cw_sb[:, ch:ch + 1],
                                                                    scalar2=None, op0=ALU.mult), reads=[f"tA{a}", "cw"], writes=[f"tB{a}"])
                    S.op("dve", lambda e, a=a, ch=ch: e.scalar_tensor_tensor(out=tB[a][:, 0:512], in0=tA[a][:, 1:513], scalar=cw_sb[:, 16 + ch:17 + ch],
                                                                           in1=tB[a][:, 0:512], op0=ALU.mult, op1=ALU.add),
                         reads=[f"tA{a}", "cw", f"tB{a}"], writes=[f"tB{a}"])
                    S.op("dve", lambda e, a=a, ch=ch: e.scalar_tensor_tensor(out=tB[a][:, 0:512], in0=tA[a][:, 2:514], scalar=cw_sb[:, 32 + ch:33 + ch],
                                                                           in1=tB[a][:, 0:512], op0=ALU.mult, op1=ALU.add),
                         reads=[f"tA{a}", "cw", f"tB{a}"], writes=[f"tB{a}"])
                    wt, wk = load_unit("win", ch)
                    bc, bh = next_pair()
                    mm_unit(wt, wk, KC, hc, hck, bc, T)
                    S.op("dve", lambda e, a=a, ch=ch, bc=bc: e.tensor_tensor(out=yAT[:, ch, :], in0=ps[bc][:, :], in1=tB[a][:, 0:512], op=ALU.mult),
                         reads=[PSK(bc), f"tB{a}"], writes=[f"yAT.{ch}"])
                for g in range(4):
                    qa = g % 2
                    qg = qTg[qa]
                    for hh in range(4):
                        wt, wk = load_unit("win", 48 + 4 * g + hh)
                        bc, bh = next_pair()
                        mm_unit(wt, wk, KC, hc, hck, bc, T)
                        S.op("act", lambda e, hh=hh, bc=bc, qg=qg: e.activation(out=qg[:, :, hh, :], in_=ps[bc][:, :].rearrange("p (a b) -> p a b", a=4), func=AF.Copy),
                             reads=[PSK(bc)], writes=[f"qTg{qa}", "mTall"])
                    for i in range(4):
                        a = cnt["t"] % 2
                        cnt["t"] += 1
                        bo, bs = (6, 7)
                        for off in range(3):
                            wb = i + off
                            b_s = off % 2
                            if j == 0 and i == 0 and off == 0:
                                tab = eeb[:, 4 * g:4 * g + 4, :]
                                tk = "eeb"
                            elif j == NT - 1 and i == 3 and off == 2:
                                tab = eeb[:, 4 * g:4 * g + 4, :]
                                tk = "eeb"
                            else:
                                tab = etb[:, off, 4 * g:4 * g + 4, :]
                                tk = "etb"
                            S.op("pe", lambda e, g=g, wb=wb, i=i, b_s=b_s, qg=qg: e.matmul(
                                ps[b_s][:, :], lhsT=kT[:, g, wb, :], rhs=qg[:, i, :, :].rearrange("p a b -> p (a b)"),
                                start=True, stop=True), reads=["kT", f"qTg{qa}", "Rall"], writes=[PSK(b_s)])
                            p_ = off % 2
                            S.op("act", lambda e, p_=p_, b_s=b_s: e.activation(out=tD[p_][:], in_=ps[b_s][:, :], func=AF.Exp, scale=SCALE),
                                 reads=[PSK(b_s)], writes=[f"tD{p_}"])
                            S.op("dve", lambda e, p_=p_, tab=tab: e.tensor_tensor(out=pT[p_][:], in0=tD[p_][:], in1=tab.rearrange("p a b -> p (a b)"), op=ALU.mult),
                                 reads=[f"tD{p_}", tk], writes=[f"pT{p_}"])
                            S.op("pe", lambda e, g=g, wb=wb, p_=p_, bo=bo, off=off: e.matmul(
                                ps[bo][:, :], lhsT=vtok[:, wb, g * 128:(g + 1) * 128], rhs=pT[p_][:], start=(off == 0), stop=(off == 2)),
                                reads=["vtok", f"pT{p_}", "Rall"], writes=[PSK(bo)])
                            S.op("pe", lambda e, p_=p_, bs=bs, off=off: e.matmul(
                                ps[bs][:, :], lhsT=ones_b[:], rhs=pT[p_][:], start=(off == 0), stop=(off == 2)),
                                reads=["ones_b", f"pT{p_}"], writes=[PSK(bs)])
                        for hh in range(4):
                            S.op("dve", lambda e, a=a, bs=bs, g=g, hh=hh: e.tensor_scalar(
                                out=tC[a][:, hh * 128:(hh + 1) * 128], in0=ps[bs][:, hh * 128:(hh + 1) * 128],
                                scalar1=esn[:, 4 * g + hh:4 * g + hh + 1], scalar2=None, op0=ALU.add),
                                reads=[PSK(bs), "esn"], writes=[f"tC{a}"])
                        S.op("dve", lambda e, a=a: e.reciprocal(out=tC[a][:], in_=tC[a][:]), reads=[f"tC{a}"], writes=[f"tC{a}"])
                        S.op("dve", lambda e, a=a, bo=bo, g=g, i=i: e.tensor_tensor(
                            out=oT[:, 4 * g:4 * g + 4, i * 128:(i + 1) * 128], in0=ps[bo][:, :].rearrange("p (a b) -> p a b", a=4),
                            in1=tC[a][:].rearrange("p (a b) -> p a b", a=4), op=ALU.mult),
                             reads=[PSK(bo), f"tC{a}"], writes=[f"oT.{4 * g}", f"oT.{4 * g + 1}", f"oT.{4 * g + 2}", f"oT.{4 * g + 3}"])
                for i in range(KC):
                    a = cnt["t"] % 2
                    cnt["t"] += 1
                    b0 = 4 * a
                    wt, wk = load_unit("win", 72 + i)
                    mm_unit(wt, wk, KC, hc, hck, b0, T)
                    S.op("act", lambda e, a=a, b0=b0: e.activation(out=tA[a][:, 0:512], in_=ps[b0][:, :], func=AF.Sigmoid),
                         reads=[PSK(b0)], writes=[f"tA{a}"])
                    wt, wk = load_unit("win", 104 + i)
                    mm_unit(wt, wk, KC, hc, hck, b0 + 1, T)
                    S.op("act", lambda e, a=a, b0=b0: e.activation(out=tB[a][:, 0:512], in_=ps[b0 + 1][:, :], func=AF.Sigmoid),
                         reads=[PSK(b0 + 1)], writes=[f"tB{a}"])
                    wt, wk = load_unit("wco", i)
                    mm_unit(wt, wk, 16, lambda k: yAT[:, k, :], lambda k: [f"yAT.{k}"], b0 + 2, T)
                    S.op("dve", lambda e, a=a, b0=b0: e.tensor_tensor(out=tA[a][:, 0:512], in0=ps[b0 + 2][:, :], in1=tA[a][:, 0:512], op=ALU.mult),
                         reads=[PSK(b0 + 2), f"tA{a}"], writes=[f"tA{a}"])
                    wt, wk = load_unit("wao", i)
                    mm_unit(wt, wk, 16, lambda k: oT[:, k, :], lambda k: [f"oT.{k}"], b0 + 3, T)
                    S.op("dve", lambda e, a=a, b0=b0: e.tensor_tensor(out=tB[a][:, 0:512], in0=ps[b0 + 3][:, :], in1=tB[a][:, 0:512], op=ALU.mult),
                         reads=[PSK(b0 + 3), f"tB{a}"], writes=[f"tB{a}"])
                    S.op("dve", lambda e, a=a, i=i: e.tensor_tensor(out=mT[:, i, :], in0=tA[a][:, 0:512], in1=tB[a][:, 0:512], op=ALU.add),
                         reads=[f"tA{a}", f"tB{a}"], writes=[f"mT.{i}", "Rall"] + (RK if i == 0 else []))

                def resid_stage(wname, nk, rhs_fn, rk_fn, src_dram, dst_dram, skey_r, skey_w, ssbank):
                    for i in range(KC):
                        o = cnt["o"] % NO
                        cnt["o"] += 1
                        bank = i % 4
                        wt, wk = load_unit(wname, i)
                        S.dma("sp", f"xo{o}", xo[o][:], src_dram(i), reads=skey_r(i), writes=[f"xo{o}"])
                        mm_unit(wt, wk, nk, rhs_fn, rk_fn, bank, T)
                        S.op("dve", lambda e, o=o, bank=bank: e.tensor_tensor(out=xo[o][:], in0=ps[bank][:, :], in1=xo[o][:], op=ALU.add),
                             reads=[PSK(bank), f"xo{o}"], writes=[f"xo{o}"])
                        a = cnt["t"] % 2
                        cnt["t"] += 1
                        S.op("act", lambda e, o=o, a=a: e.activation(out=tD[a][:], in_=xo[o][:], func=AF.Square),
                             reads=[f"xo{o}"], writes=[f"tD{a}"])
                        S.op("pe", lambda e, a=a, i=i: e.matmul(ps[ssbank][:, :], lhsT=ones_f[:], rhs=tD[a][:], start=(i == 0), stop=(i == KC - 1)),
                             reads=[f"tD{a}", "ones_f"], writes=[PSK(ssbank)])
                        S.dma("sp", f"xo{o}", dst_dram(i), xo[o][:], reads=[f"xo{o}"], writes=skey_w(i))

                resid_stage("wout", KC, lambda k: mT[:, k, :], lambda k: [f"mT.{k}", "mTall"],
                            lambda i: xT[i * 128:(i + 1) * 128, c0 + 128:c0 + 128 + T], lambda i: x1s[i * 128:(i + 1) * 128, tcol],
                            lambda i: [], lambda i: [f"x1s.{i}"], 6)
                rms_finish(ps[6][:, :], rstd[:, 0:T], PSK(6), "rstd0")
                for c in range(KC):
                    o = cnt["o"] % NO
                    cnt["o"] += 1
                    S.dma("sp", f"xo{o}", xo[o][:], x1s[c * 128:(c + 1) * 128, tcol], reads=[f"x1s.{c}"], writes=[f"xo{o}"])
                    S.op("dve", lambda e, o=o, c=c: e.scalar_tensor_tensor(out=hTc[:, c, :], in0=xo[o][:], scalar=g_sb[:, KC + c:KC + c + 1],
                                                                         in1=rstd[:, 0:T], op0=ALU.mult, op1=ALU.mult),
                         reads=[f"xo{o}", "g", "rstd0"], writes=[f"hTc.{c}"])
                for h in range(4):
                    wt, wk = load_unit("wq", h)
                    mm_unit(wt, wk, KC, hc, hck, h % 2, T)
                    S.op("act", lambda e, h=h: e.activation(out=qcT[:, h, :], in_=ps[h % 2][:, :], func=AF.Copy),
                         reads=[PSK(h % 2)], writes=[f"qcT.{h}"])
                for h in range(4):
                    a = h % 2
                    bo, bs = (2, 3) if a == 0 else (4, 5)
                    for mb in range(2):
                        b_s = mb
                        S.op("pe", lambda e, h=h, mb=mb, b_s=b_s: e.matmul(ps[b_s][:, :], lhsT=kcT[:, h, mb * 128:(mb + 1) * 128], rhs=qcT[:, h, :],
                                                                         start=True, stop=True), reads=["kcT", f"qcT.{h}"], writes=[PSK(b_s)])
                        S.op("act", lambda e, mb=mb, b_s=b_s: e.activation(out=pT[mb][:], in_=ps[b_s][:, :], func=AF.Exp, scale=SCALE),
                             reads=[PSK(b_s)], writes=[f"pT{mb}"])
                        S.op("pe", lambda e, h=h, mb=mb, bo=bo: e.matmul(ps[bo][:, :], lhsT=vc[:, mb, h * 128:(h + 1) * 128], rhs=pT[mb][:],
                                                                        start=(mb == 0), stop=(mb == 1)), reads=["vc", f"pT{mb}"], writes=[PSK(bo)])
                        S.op("pe", lambda e, mb=mb, bs=bs: e.matmul(ps[bs][:, :], lhsT=ones_b[:], rhs=pT[mb][:],
                                                                   start=(mb == 0), stop=(mb == 1)), reads=["ones_b", f"pT{mb}"], writes=[PSK(bs)])
                    S.op("dve", lambda e, a=a, bs=bs: e.reciprocal(out=tC[a][:], in_=ps[bs][:, :]), reads=[PSK(bs)], writes=[f"tC{a}"])
                    S.op("dve", lambda e, a=a, bo=bo, h=h: e.tensor_tensor(out=ocT[:, h, :], in0=ps[bo][:, :], in1=tC[a][:], op=ALU.mult),
                         reads=[PSK(bo), f"tC{a}"], writes=[f"ocT.{h}"])
                resid_stage("wo", 4, lambda k: ocT[:, k, :], lambda k: [f"ocT.{k}"],
                            lambda i: x1s[i * 128:(i + 1) * 128, tcol], lambda i: x2s[i * 128:(i + 1) * 128, tcol],
                            lambda i: [f"x1s.{i}"], lambda i: [f"x2s.{j}.{i}"], 6)
                rms_finish(ps[6][:, :], rstd[:, 0:T], PSK(6), "rstd0")
                for c in range(KC):
                    o = cnt["o"] % NO
                    cnt["o"] += 1
                    S.dma("sp", f"xo{o}", xo[o][:], x2s[c * 128:(c + 1) * 128, tcol], reads=[f"x2s.{j}.{c}"], writes=[f"xo{o}"])
                    S.op("dve", lambda e, o=o, c=c: e.scalar_tensor_tensor(out=xo[o][:], in0=xo[o][:], scalar=g_sb[:, 2 * KC + c:2 * KC + c + 1],
                                                                         in1=rstd[:, 0:T], op0=ALU.mult, op1=ALU.mult),
                         reads=[f"xo{o}", "g", "rstd0"], writes=[f"xo{o}"])
                    S.op("pe", lambda e, o=o, c=c: e.matmul(ps[7][0:16, :], lhsT=wr_sb[:, c * 16:(c + 1) * 16], rhs=xo[o][:],
                                                            start=(c == 0), stop=(c == KC - 1)), reads=["wr", f"xo{o}"], writes=[PSK(7)])
                    S.op("act", lambda e, o=o: e.activation(out=hb[o][:], in_=xo[o][:], func=AF.Copy), reads=[f"xo{o}"], writes=[f"hb{o}"])
                    S.dma("sp", f"hb{o}", h3s[c * 128:(c + 1) * 128, tcol], hb[o][:], reads=[f"hb{o}"], writes=[f"h3s.{j}.{c}"])
                S.op("act", lambda e: e.activation(out=lgs[:], in_=ps[7][0:16, :], func=AF.Exp), reads=[PSK(7)], writes=["lgs"])
                S.op("pe", lambda e: e.matmul(ps[0][0:16, :], lhsT=ones_f[0:16, 0:16], rhs=lgs[:], start=True, stop=True),
                     reads=["lgs", "ones_f"], writes=[PSK(0)])
                S.op("dve", lambda e: e.reciprocal(out=tC[0][0:16, :], in_=ps[0][0:16, :]), reads=[PSK(0)], writes=["tC0"])
                S.op("dve", lambda e: e.tensor_tensor(out=afft[:], in0=lgs[:], in1=tC[0][0:16, :], op=ALU.mult),
                     reads=["lgs", "tC0"], writes=["afft"])
                S.dma("sp", "afft", aff_loc[:, tcol], afft[:], reads=["afft"], writes=["aff_loc"])
            S.barrier()
            S.flush(blk)
        S.cc(lambda e: e.collective_compute("AllGather", ALU.bypass, replica_groups=[list(range(NCORES))],
                                            ins=[aff_loc], outs=[aff_all]), ["aff_all"])
        for e_ in range(NEARLY, NE):
            cc_op("wg", e_)
            cc_op("wu", e_)
            cc_op("wd", e_)
        es3 = ExitStack()
        with es3:
            def sb3(name, shape, dt=F32):
                return es3.enter_context(nc.sbuf_tensor(name, list(shape), dt))

            A0 = sb3("A0", [128, 1024])
            A1 = sb3("A1", [128, 1024])
            junk = sb3("junk", [128, 1024])
            lo = sb3("lo", [128, 1])
            hi = sb3("hi", [128, 1])
            mid = sb3("mid", [128, 1])
            cn = sb3("cn", [128, 1])
            ge = sb3("ge", [128, 1])
            dl = sb3("dl", [128, 1])
            thr = sb3("thr", [16, 1])
            bsel = sb3("bsel_sb", [16, 1])
            sel_sb = sb3("sel_sb", [16, 16 * 128])
            aloc = sb3("aloc", [16, TOK])
            gmk = sb3("gmk", [16, TOK])
            acc = sb3("acc", [128, KC, T])
            h3T = sb3("h3T", [128, KC, T], BF16)
            hid = sb3("hid", [128, 16, T], BF16)
            gmb = [sb3(f"gmb{i}", [128, T]) for i in range(2)]
            sl = [sb3(f"sl{i}", [128, T]) for i in range(2)]
            tt = [sb3(f"tt{i}", [128, T]) for i in range(2)]
            sq2 = [sb3(f"sq2{i}", [128, T]) for i in range(2)]
            ot = [sb3(f"ot{i}", [128, T]) for i in range(2)]
            rs2 = sb3("rs2", [128, T])
            for nm in ("A0", "A1", "acc", "h3T", "ot0", "ot1"):
                dlane(nm)
            S.dma("sp", "misc", bsel[:], bsel_d, writes=["bsel"])
            S.dma("sp", "misc", sel_sb[:], selm, writes=["sel"])
            S.dma("sp", "misc", aloc[:], aff_loc, reads=["aff_loc"], writes=["aloc"])
            for b, Ab in ((0, A0), (1, A1)):
                for g in range(8):
                    r = 4 * b + g // 2
                    S.dma("sp", f"A{b}", Ab[g * 16:(g + 1) * 16, :], aff_all[r * 16:(r + 1) * 16, (g % 2) * 1024:(g % 2 + 1) * 1024],
                          reads=["aff_all"], writes=[f"A{b}"])
            S.seal("misc", ["bsel", "sel", "aloc"])
            thr_b = []
            for b, Ab in ((0, A0), (1, A1)):
                S.op("dve", lambda e: e.memset(lo[:], 0.0), writes=["lo"])
                S.op("dve", lambda e: e.memset(hi[:], 1.0), writes=["hi"])
                for itn in range(NBIS):
                    S.op("dve", lambda e: e.tensor_tensor(out=mid[:], in0=lo[:], in1=hi[:], op=ALU.add), reads=["lo", "hi"], writes=["mid"])
                    S.op("dve", lambda e: e.tensor_scalar(out=mid[:], in0=mid[:], scalar1=0.5, scalar2=None, op0=ALU.mult), reads=["mid"], writes=["mid"])
                    S.op("dve", lambda e, Ab=Ab: e.tensor_scalar(out=junk[:], in0=Ab[:], scalar1=mid[:, 0:1], scalar2=None, op0=ALU.is_ge),
                         reads=[f"A{b}", "mid"], writes=["junk"])
                    S.op("dve", lambda e: e.tensor_reduce(out=cn[:], in_=junk[:], axis=mybir.AxisListType.X, op=ALU.add), reads=["junk"], writes=["cn"])
                    S.op("pe", lambda e: e.matmul(ps[0][:, 0:1], lhsT=gmat_sb[:], rhs=cn[:], start=True, stop=True), reads=["gmat", "cn"], writes=[PSK(0)])
                    S.op("dve", lambda e: e.tensor_scalar(out=ge[:], in0=ps[0][:, 0:1], scalar1=float(CAP) - 0.5, scalar2=None, op0=ALU.is_ge),
                         reads=[PSK(0)], writes=["ge"])
                    S.op("dve", lambda e: e.tensor_tensor(out=dl[:], in0=mid[:], in1=lo[:], op=ALU.subtract), reads=["mid", "lo"], writes=["dl"])
                    S.op("dve", lambda e: e.tensor_tensor(out=dl[:], in0=dl[:], in1=ge[:], op=ALU.mult), reads=["dl", "ge"], writes=["dl"])
                    S.op("dve", lambda e: e.tensor_tensor(out=lo[:], in0=lo[:], in1=dl[:], op=ALU.add), reads=["lo", "dl"], writes=["lo"])
                    S.op("dve", lambda e: e.tensor_tensor(out=dl[:], in0=hi[:], in1=mid[:], op=ALU.subtract), reads=["hi", "mid"], writes=["dl"])
                    S.op("dve", lambda e: e.tensor_tensor(out=dl[:], in0=dl[:], in1=ge[:], op=ALU.mult), reads=["dl", "ge"], writes=["dl"])
                    S.op("dve", lambda e: e.tensor_tensor(out=hi[:], in0=mid[:], in1=dl[:], op=ALU.add), reads=["mid", "dl"], writes=["hi"])
                tb = sb3(f"thr{b}", [16, 1])
                S.op("dve", lambda e, tb=tb: e.tensor_copy(out=tb[:], in_=lo[0:16, :]), reads=["lo"], writes=[f"thr{b}"])
                thr_b.append(tb)
            S.op("dve", lambda e: e.tensor_tensor(out=thr[:], in0=thr_b[1][:], in1=thr_b[0][:], op=ALU.subtract), reads=["thr0", "thr1"], writes=["thr"])
            S.op("dve", lambda e: e.tensor_tensor(out=thr[:], in0=thr[:], in1=bsel[:], op=ALU.mult), reads=["thr", "bsel"], writes=["thr"])
            S.op("dve", lambda e: e.tensor_tensor(out=thr[:], in0=thr[:], in1=thr_b[0][:], op=ALU.add), reads=["thr", "thr0"], writes=["thr"])
            S.op("dve", lambda e: e.scalar_tensor_tensor(out=gmk[:], in0=aloc[:], scalar=thr[:, 0:1], in1=aloc[:], op0=ALU.is_ge, op1=ALU.mult),
                 reads=["aloc", "thr"], writes=["gmk"])

            cm = {"t": 0}
            for j in range(NT):
                tcol = slice(j * T, (j + 1) * T)
                for c in range(KC):
                    S.dma("sp", "acc", acc[:, c, :], x2s[c * 128:(c + 1) * 128, tcol], reads=[f"x2s.{j}.{c}"], writes=[f"acc.{c}"])
                    S.dma("sp", "h3T", h3T[:, c, :], h3s[c * 128:(c + 1) * 128, tcol], reads=[f"h3s.{j}.{c}"], writes=[f"h3T.{c}"])
                S.seal("acc", [f"acc.{c}" for c in range(KC)])
                S.seal("h3T", [f"h3T.{c}" for c in range(KC)])
                for ex in range(NE):
                    gi = ex % 2
                    S.op("pe", lambda e, ex=ex, tcol=tcol: e.matmul(ps[6][:, :], lhsT=sel_sb[:, ex * 128:(ex + 1) * 128], rhs=gmk[:, tcol], start=True, stop=True),
                         reads=["sel", "gmk"], writes=[PSK(6)])
                    S.op("act", lambda e, gi=gi: e.activation(out=gmb[gi][:], in_=ps[6][:, :], func=AF.Copy), reads=[PSK(6)], writes=[f"gmb{gi}"])
                    for f in range(16):
                        a = cm["t"] % 2
                        cm["t"] += 1
                        ba, bu = (0, 1) if a == 0 else (2, 3)
                        wt, wk = load_unit("wg", ex * 16 + f)
                        for k in range(KC):
                            S.op("pe", lambda e, k=k, ba=ba, wt=wt: e.matmul(ps[ba][:, :], lhsT=wt[:, k * 128:(k + 1) * 128], rhs=h3T[:, k, :],
                                                                            start=(k == 0), stop=(k == KC - 1)), reads=[wk, f"h3T.{k}"], writes=[PSK(ba)], sig=(k == KC - 1))
                        S.op("act", lambda e, a=a, ba=ba: e.activation(out=sl[a][:], in_=ps[ba][:, :], func=AF.Silu), reads=[PSK(ba)], writes=[f"sl{a}"])
                        wt, wk = load_unit("wu", ex * 16 + f)
                        for k in range(KC):
                            S.op("pe", lambda e, k=k, bu=bu, wt=wt: e.matmul(ps[bu][:, :], lhsT=wt[:, k * 128:(k + 1) * 128], rhs=h3T[:, k, :],
                                                                            start=(k == 0), stop=(k == KC - 1)), reads=[wk, f"h3T.{k}"], writes=[PSK(bu)], sig=(k == KC - 1))
                        S.op("dve", lambda e, a=a, bu=bu: e.tensor_tensor(out=tt[a][:], in0=ps[bu][:, :], in1=sl[a][:], op=ALU.mult),
                             reads=[PSK(bu), f"sl{a}"], writes=[f"tt{a}"])
                        S.op("dve", lambda e, a=a, f=f, gi=gi: e.tensor_tensor(out=hid[:, f, :], in0=tt[a][:], in1=gmb[gi][:], op=ALU.mult),
                             reads=[f"tt{a}", f"gmb{gi}"], writes=[f"hid.{f}"])
                    for i in range(KC):
                        bank = 4 + (i % 2)
                        wt, wk = load_unit("wd", ex * 32 + i)
                        for k in range(16):
                            S.op("pe", lambda e, k=k, bank=bank, wt=wt: e.matmul(ps[bank][:, :], lhsT=wt[:, k * 128:(k + 1) * 128], rhs=hid[:, k, :],
                                                                                start=(k == 0), stop=(k == 15)), reads=[wk, f"hid.{k}"], writes=[PSK(bank)], sig=(k == 15))
                        S.op("dve", lambda e, i=i, bank=bank: e.tensor_tensor(out=acc[:, i, :], in0=ps[bank][:, :], in1=acc[:, i, :], op=ALU.add),
                             reads=[PSK(bank), f"acc.{i}"], writes=[f"acc.{i}"])
                for c in range(KC):
                    a = c % 2
                    S.op("act", lambda e, a=a, c=c: e.activation(out=sq2[a][:], in_=acc[:, c, :], func=AF.Square), reads=[f"acc.{c}"], writes=[f"sq2{a}"])
                    S.op("pe", lambda e, a=a, c=c: e.matmul(ps[7][:, :], lhsT=ones_f[:], rhs=sq2[a][:], start=(c == 0), stop=(c == KC - 1)),
                         reads=[f"sq2{a}", "ones_f"], writes=[PSK(7)])
                rms_finish(ps[7][:, :], rs2[:], PSK(7), "rs2")
                for c in range(KC):
                    a = c % 2
                    S.op("dve", lambda e, a=a, c=c: e.scalar_tensor_tensor(out=ot[a][:], in0=acc[:, c, :], scalar=g_sb[:, 3 * KC + c:3 * KC + c + 1],
                                                                         in1=rs2[:], op0=ALU.mult, op1=ALU.mult),
                         reads=[f"acc.{c}", "g", "rs2"], writes=[f"ot{a}"])
                    S.dma("sp", f"ot{a}", outT[c * 128:(c + 1) * 128, tcol], ot[a][:], reads=[f"ot{a}"], writes=[f"out.{j}.{c}"])
            S.barrier()
            S.flush(blk)
    return nc


def _units(Wm, kcn):
    K, N = Wm.shape
    U = N // 128
    return np.ascontiguousarray(Wm.reshape(kcn, 128, U, 128).transpose(2, 1, 0, 3)).reshape(U, 128, kcn * 128)


_NC_CACHE = {}


def kernel(x, mem, g_mix, w_in, conv_w, attn_sinks, w_conv_out, w_attn_out, w_out,
           g_cross, g_mem, w_q_cross, w_kv_cross, w_o_cross,
           g_moe, w_router, w_gate_e, w_up_e, w_down_e, g_final):
    import ml_dtypes
    f = lambda a: np.asarray(a, dtype=np.float32)
    x, mem = f(x), f(mem)
    B = x.shape[0]
    xp = np.zeros((B, SEQ + 256, D), np.float32)
    xp[:, 128:128 + SEQ] = x

    def gl(g):
        return f(g).reshape(KC, 128).T

    gains = np.ascontiguousarray(np.concatenate([gl(g_mix), gl(g_cross), gl(g_moe), gl(g_final)], axis=1))
    gmem = np.ascontiguousarray(gl(g_mem))
    convw = np.ascontiguousarray(f(conv_w).reshape(3, 16, 128).transpose(2, 0, 1)).reshape(128, 48)
    sinks = np.ascontiguousarray(np.broadcast_to(f(attn_sinks)[None, :], (128, 16)))
    slopes = 2.0 ** (-8.0 * (np.arange(16, dtype=np.float64) + 1.0) / 16.0)
    kk = np.arange(128)[:, None, None, None]
    oo = np.arange(3)[None, :, None, None]
    qq = np.arange(128)[None, None, None, :]
    dist = np.abs((oo - 1) * 128 + kk - qq).astype(np.float64)
    E = np.where(dist <= 128, np.exp(-slopes[None, None, :, None] * dist), 0.0)
    etab = E.reshape(128, -1).astype(ml_dtypes.bfloat16)
    wrt = np.ascontiguousarray(f(w_router).reshape(KC, 128, 16).transpose(1, 0, 2)).reshape(128, KC * 16)
    selm = np.zeros((16, 16, 128), np.float32)
    for e in range(16):
        selm[e, e, :] = 1.0
    selm = selm.reshape(16, 2048)
    gmat = (np.arange(128)[:, None] % 16 == np.arange(128)[None, :] % 16).astype(np.float32)

    def shard_il(units):
        return [np.ascontiguousarray(units[r::8]).reshape(-1, units.shape[2]) for r in range(NCORES)]

    win_s = shard_il(_units(f(w_in), 32))
    wco_s = shard_il(_units(f(w_conv_out), 16))
    wao_s = shard_il(_units(f(w_attn_out), 16))
    wout_s = shard_il(_units(f(w_out), 32))
    wo_s = shard_il(_units(f(w_o_cross), 4))
    uq = _units(f(w_q_cross), 32).reshape(4, 128, 2, 2048).transpose(0, 2, 1, 3).reshape(8, 128, 2048)
    wq_s = [np.ascontiguousarray(uq[r]) for r in range(NCORES)]
    ukv = _units(f(w_kv_cross), 32)
    wkv_s = [np.ascontiguousarray(ukv[r]) for r in range(NCORES)]
    wg_s = [np.empty((16 * 256, 4096), np.float32) for _ in range(NCORES)]
    wu_s = [np.empty((16 * 256, 4096), np.float32) for _ in range(NCORES)]
    wd_s = [np.empty((16 * 512, 2048), np.float32) for _ in range(NCORES)]
    for e in range(NE):
        ug = _units(f(w_gate_e[e]), 32)
        uu = _units(f(w_up_e[e]), 32)
        ud = _units(f(w_down_e[e]), 16)
        for r in range(NCORES):
            wg_s[r][e * 256:(e + 1) * 256] = ug[2 * r:2 * r + 2].reshape(256, 4096)
            wu_s[r][e * 256:(e + 1) * 256] = uu[2 * r:2 * r + 2].reshape(256, 4096)
            wd_s[r][e * 512:(e + 1) * 512] = ud[4 * r:4 * r + 4].reshape(512, 2048)

    in_maps = []
    for c in range(NCORES):
        b, q = c // 4, c % 4
        t0 = q * TOK
        ee = np.zeros((128, 2, 16, 128), np.float64)
        if q != 0:
            ee[:, 0] = E[:, 0]
        if q != 3:
            ee[:, 1] = E[:, 2]
        in_maps.append({
            "xT": np.ascontiguousarray(xp[b, t0:t0 + TOK + 256].T),
            "memT": np.ascontiguousarray(mem[b].T),
            "gains": gains, "gmem": gmem, "convw": convw, "sinks": sinks,
            "etab": etab, "eedge": ee.reshape(128, -1).astype(ml_dtypes.bfloat16),
            "wrt": wrt, "selm": selm, "gmat": gmat,
            "bsel": np.full((16, 1), float(b), np.float32),
            "win_sh": win_s[c], "wco_sh": wco_s[c], "wao_sh": wao_s[c], "wout_sh": wout_s[c],
            "wq_sh": wq_s[c], "wkv_sh": wkv_s[c], "wo_sh": wo_s[c],
            "wg_sh": wg_s[c], "wu_sh": wu_s[c], "wd_sh": wd_s[c],
        })
    if "nc" not in _NC_CACHE:
        _NC_CACHE["nc"] = build_nc()
    res = run_bass_kernel_spmd(_NC_CACHE["nc"], in_maps, core_ids=list(range(NCORES)))
    out = np.empty((B, SEQ, D), np.float32)
    for c in range(NCORES):
        b, q = c // 4, c % 4
        out[b, q * TOK:(q + 1) * TOK, :] = res.results[c]["outT"].T
    return out
```
